# Optimizing a Trainium2 kernel written in Bass

```python
import jax
import jax.numpy as jnp
from jax import lax
import numpy as np

D_MODEL = 2048
BATCH = 1
SEQ = 16384
DEPTH = 1

N_MEM = 256
HEAD_DIM = 128
EPS = 1e-6
N_HEADS_A = (D_MODEL // 2) // HEAD_DIM
DILATED_PATTERNS = ((128, 1), (512, 4), (2048, 16))
ATTN_BLOCK = 128
N_HEADS_B = (D_MODEL // 2) // HEAD_DIM
DK_B = HEAD_DIM
DV_B = HEAD_DIM
CONV_WIDTH = 4
DELTA_CHUNK = 64
W_A = N_HEADS_A * HEAD_DIM
W_BK = N_HEADS_B * DK_B
W_BV = N_HEADS_B * DV_B
MIX_WIDTH = W_A + W_BV
D_IN = 3 * W_A + 2 * W_BK + 2 * W_BV + 2 * N_HEADS_B
N_HEADS_MEM = 4
N_GROUPS = 8
EXPERTS_PER_GROUP = 8
N_EXPERTS = N_GROUPS * EXPERTS_PER_GROUP
TOP_K_EXPERT = 2
D_EXPERT = D_MODEL // 4
MOE_BLOCK = 128

kernel_name = 'hybrid_dilated_deltanet_hmoe'


def rms_norm(x, g):
    x32 = x.astype(jnp.float32)
    return x32 * lax.rsqrt(jnp.mean(x32 * x32, axis=-1, keepdims=True) + EPS) * g.astype(jnp.float32)


def l2_normalize(x):
    return x * lax.rsqrt(jnp.sum(x * x, axis=-1, keepdims=True) + EPS)


def alibi_slopes(n_heads):
    return jnp.asarray(2.0 ** (-8.0 * np.arange(1, n_heads + 1) / n_heads), dtype=jnp.float32)


def dilated_window_attn(q, k, v, slopes, window, dilation):
    B, S, H, Dh = q.shape
    d = dilation
    n_back = window // d
    L = S // d
    nb = -(-L // ATTN_BLOCK)
    Lp = nb * ATTN_BLOCK
    Z = B * d

    def by_residue(t):
        t = t.reshape(B, L, d, H, Dh).transpose(0, 2, 3, 1, 4).reshape(Z, H, L, Dh)
        return jnp.pad(t, ((0, 0), (0, 0), (0, Lp - L), (0, 0)))

    def band(t):
        tb = t.reshape(Z, H, nb, ATTN_BLOCK, Dh)
        prev = jnp.pad(tb, ((0, 0), (0, 0), (1, 0), (0, 0), (0, 0)))[:, :, :-1]
        return jnp.concatenate([prev, tb], axis=3)

    qb = by_residue(q).reshape(Z, H, nb, ATTN_BLOCK, Dh)
    kb = band(by_residue(k))
    vb = band(by_residue(v))
    s = jnp.einsum('zhnqd,zhnkd->zhnqk', qb, kb) * (Dh ** -0.5)
    qi = jnp.arange(ATTN_BLOCK)[:, None] + ATTN_BLOCK
    ki = jnp.arange(2 * ATTN_BLOCK)[None, :]
    dist = qi - ki
    key_idx = (jnp.arange(nb) * ATTN_BLOCK - ATTN_BLOCK)[:, None, None] + ki[None]
    valid = (dist >= 0) & (dist <= n_back) & (key_idx >= 0)
    bias = -slopes[:, None, None, None] * (d * dist).astype(jnp.float32)
    s = jnp.where(valid, s + bias, -jnp.inf)
    lse = jax.nn.logsumexp(s, axis=-1)
    p = jnp.exp(s - lse[..., None])
    o = jnp.einsum('zhnqk,zhnkd->zhnqd', p, vb)
    o = o.reshape(Z, H, Lp, Dh)[:, :, :L].reshape(B, d, H, L, Dh).transpose(0, 3, 1, 2, 4).reshape(B, S, H, Dh)
    lse = lse.reshape(Z, H, Lp)[:, :, :L].reshape(B, d, H, L).transpose(0, 3, 1, 2).reshape(B, S, H)
    return o, lse


def dilated_attention(q, k, v):
    slopes = alibi_slopes(q.shape[2])
    outs = []
    lses = []
    for window, dilation in DILATED_PATTERNS:
        o, lse = dilated_window_attn(q, k, v, slopes, window, dilation)
        outs.append(o)
        lses.append(lse)
    w = jax.nn.softmax(jnp.stack(lses, axis=0), axis=0)
    return jnp.sum(w[..., None] * jnp.stack(outs, axis=0), axis=0)


def causal_depthwise_conv(u, w):
    C = u.shape[-1]
    return lax.conv_general_dilated(u, w[:, None, :].astype(u.dtype), (1,), [(CONV_WIDTH - 1, 0)],
                                    dimension_numbers=('NWC', 'WIO', 'NWC'), feature_group_count=C)


def gated_delta_rule(q, k, v, g, beta):
    B, S, H, dk = q.shape
    dv = v.shape[-1]
    C = DELTA_CHUNK
    N = S // C

    def chunks(t):
        return jnp.moveaxis(t.reshape((B, N, C, H) + t.shape[3:]), 3, 1)

    q = chunks(q * (dk ** -0.5))
    k = chunks(k)
    v = chunks(v)
    beta = chunks(beta)
    g = jnp.cumsum(chunks(g), axis=-1)
    causal = jnp.tril(jnp.ones((C, C), dtype=bool))
    strict = jnp.tril(jnp.ones((C, C), dtype=bool), -1)
    decay = jnp.exp(jnp.where(causal, g[..., :, None] - g[..., None, :], -jnp.inf))
    kb = k * beta[..., None]
    a_mat = jnp.where(strict, jnp.einsum('bhncd,bhnsd->bhncs', kb, k) * decay, 0.0)
    eye = jnp.eye(C, dtype=a_mat.dtype)
    t_mat = lax.linalg.triangular_solve(eye + a_mat, jnp.broadcast_to(eye, a_mat.shape),
                                        left_side=True, lower=True, unit_diagonal=True)
    u = jnp.einsum('bhncs,bhnsv->bhncv', t_mat, v * beta[..., None])
    w = jnp.einsum('bhncs,bhnsk->bhnck', t_mat, kb * jnp.exp(g)[..., None])
    qk = jnp.einsum('bhncd,bhnsd->bhncs', q, k) * decay
    g_last = g[..., -1]
    q_dec = q * jnp.exp(g)[..., None]
    k_dec = k * jnp.exp(g_last[..., None] - g)[..., None]

    def step(state, inp):
        q_i, k_i, u_i, w_i, qk_i, gl_i = inp
        v_new = u_i - jnp.einsum('bhck,bhkv->bhcv', w_i, state)
        o_i = jnp.einsum('bhck,bhkv->bhcv', q_i, state) + jnp.einsum('bhcs,bhsv->bhcv', qk_i, v_new)
        state = state * jnp.exp(gl_i)[..., None, None] + jnp.einsum('bhck,bhcv->bhkv', k_i, v_new)
        return state, o_i

    xs = (jnp.moveaxis(q_dec, 2, 0), jnp.moveaxis(k_dec, 2, 0), jnp.moveaxis(u, 2, 0),
          jnp.moveaxis(w, 2, 0), jnp.moveaxis(qk, 2, 0), jnp.moveaxis(g_last, 2, 0))
    state0 = jnp.zeros((B, H, dk, dv), dtype=jnp.float32)
    _, o = lax.scan(step, state0, xs)
    return o.transpose(1, 0, 3, 2, 4).reshape(B, S, H, dv)


def gated_deltanet(qkv, z, b, a, conv_w, a_log, dt_bias, g_out):
    B, S, _ = qkv.shape
    qkv = jax.nn.silu(causal_depthwise_conv(qkv, conv_w))
    q, k, v = jnp.split(qkv, [W_BK, 2 * W_BK], axis=-1)
    q = l2_normalize(q.reshape(B, S, N_HEADS_B, DK_B))
    k = l2_normalize(k.reshape(B, S, N_HEADS_B, DK_B))
    v = v.reshape(B, S, N_HEADS_B, DV_B)
    beta = jax.nn.sigmoid(b)
    g = -jnp.exp(a_log.astype(jnp.float32)) * jax.nn.softplus(a + dt_bias)
    o = gated_delta_rule(q, k, v, g, beta)
    o = rms_norm(o, g_out) * jax.nn.silu(z.reshape(B, S, N_HEADS_B, DV_B))
    return o.reshape(B, S, W_BV)


def memory_cross_attention(h, mem_n, w_q, w_kv, w_o):
    B, S, _ = h.shape
    M = mem_n.shape[1]
    q = (h @ w_q).reshape(B, S, N_HEADS_MEM, HEAD_DIM)
    k, v = jnp.split(mem_n @ w_kv, 2, axis=-1)
    k = k.reshape(B, M, N_HEADS_MEM, HEAD_DIM)
    v = v.reshape(B, M, N_HEADS_MEM, HEAD_DIM)
    p = jax.nn.softmax(jnp.einsum('bshd,bmhd->bhsm', q, k) * (HEAD_DIM ** -0.5), axis=-1)
    o = jnp.einsum('bhsm,bmhd->bshd', p, v).reshape(B, S, N_HEADS_MEM * HEAD_DIM)
    return o @ w_o


def hierarchical_moe(h, w_group, b_group, w_expert, b_expert, w_gate, w_up, w_down):
    B, S, D = h.shape
    T = B * S
    t = h.reshape(T, D)
    g_logits = t @ w_group + b_group
    g_sel = jnp.argmax(g_logits, axis=-1)
    g_gate = jnp.take_along_axis(jax.nn.softmax(g_logits, axis=-1), g_sel[:, None], axis=1)
    e_logits = (t @ w_expert + b_expert).reshape(T, N_GROUPS, EXPERTS_PER_GROUP)
    e_logits = jnp.take_along_axis(e_logits, g_sel[:, None, None], axis=1)[:, 0]
    top_v, top_i = lax.top_k(e_logits, TOP_K_EXPERT)
    weights = g_gate * jax.nn.softmax(top_v, axis=-1)
    expert_id = g_sel[:, None] * EXPERTS_PER_GROUP + top_i

    M = T * TOP_K_EXPERT
    e_flat = expert_id.reshape(M)
    w_flat = weights.reshape(M)
    tok_flat = jnp.repeat(jnp.arange(T, dtype=jnp.int32), TOP_K_EXPERT)
    order = jnp.argsort(e_flat)
    e_sorted = e_flat[order]
    counts = jnp.bincount(e_flat, length=N_EXPERTS)
    padded = ((counts + MOE_BLOCK - 1) // MOE_BLOCK) * MOE_BLOCK
    start = jnp.cumsum(counts) - counts
    pstart = jnp.cumsum(padded) - padded
    dest = pstart[e_sorted] + jnp.arange(M) - start[e_sorted]
    P = (-(-M // MOE_BLOCK)) * MOE_BLOCK + N_EXPERTS * MOE_BLOCK
    n_blocks = P // MOE_BLOCK
    buf_tok = jnp.full((P,), T, dtype=jnp.int32).at[dest].set(tok_flat[order])
    buf_w = jnp.zeros((P,), dtype=t.dtype).at[dest].set(w_flat[order])
    block_expert = jnp.minimum(jnp.searchsorted(jnp.cumsum(padded), jnp.arange(n_blocks) * MOE_BLOCK, side='right'),
                               N_EXPERTS - 1)
    t_pad = jnp.concatenate([t, jnp.zeros((1, D), dtype=t.dtype)], axis=0)
    xb = t_pad[buf_tok].reshape(n_blocks, MOE_BLOCK, D)

    def expert_block(args):
        x_blk, e = args
        hid = jax.nn.silu(x_blk @ w_gate[e]) * (x_blk @ w_up[e])
        return hid @ w_down[e]

    yb = lax.map(expert_block, (xb, block_expert)).reshape(P, D)
    y = jnp.zeros((T + 1, D), dtype=yb.dtype).at[buf_tok].add(yb * buf_w[:, None])[:T]
    return y.reshape(B, S, D)


def setup_inputs(seed: int = 0) -> dict:
    key = jax.random.key(seed)
    ks = jax.random.split(key, 26)
    L = DEPTH
    f32 = jnp.float32

    def nrm(k, shape, scale):
        return jax.random.normal(k, shape, f32) * scale

    def gain(k, shape):
        return 1.0 + 0.02 * jax.random.normal(k, shape, f32)

    dt = jnp.exp(jax.random.uniform(ks[6], (L, N_HEADS_B), f32, minval=np.log(1e-3), maxval=np.log(1e-1)))
    return {
        'x': nrm(ks[0], (BATCH, SEQ, D_MODEL), 1.0),
        'mem': nrm(ks[1], (BATCH, N_MEM, D_MODEL), 1.0),
        'g_mix': gain(ks[2], (L, D_MODEL)),
        'w_in': nrm(ks[3], (L, D_MODEL, D_IN), D_MODEL ** -0.5),
        'conv_w': nrm(ks[4], (L, CONV_WIDTH, W_BK + W_BK + W_BV), CONV_WIDTH ** -0.5),
        'a_log': jnp.log(jax.random.uniform(ks[5], (L, N_HEADS_B), f32, minval=1.0, maxval=16.0)),
        'dt_bias': dt + jnp.log(-jnp.expm1(-dt)),
        'g_delta_out': gain(ks[7], (L, DV_B)),
        'g_attn_out': gain(ks[8], (L, W_A)),
        'w_out': nrm(ks[9], (L, MIX_WIDTH, D_MODEL), MIX_WIDTH ** -0.5),
        'g_cross': gain(ks[10], (L, D_MODEL)),
        'g_mem': gain(ks[11], (L, D_MODEL)),
        'w_q_mem': nrm(ks[12], (L, D_MODEL, N_HEADS_MEM * HEAD_DIM), D_MODEL ** -0.5),
        'w_kv_mem': nrm(ks[13], (L, D_MODEL, 2 * N_HEADS_MEM * HEAD_DIM), D_MODEL ** -0.5),
        'w_o_mem': nrm(ks[14], (L, N_HEADS_MEM * HEAD_DIM, D_MODEL), (N_HEADS_MEM * HEAD_DIM) ** -0.5),
        'g_moe': gain(ks[15], (L, D_MODEL)),
        'w_group': nrm(ks[16], (L, D_MODEL, N_GROUPS), D_MODEL ** -0.5),
        'b_group': nrm(ks[17], (L, N_GROUPS), 0.01),
        'w_expert': nrm(ks[18], (L, D_MODEL, N_EXPERTS), D_MODEL ** -0.5),
        'b_expert': nrm(ks[19], (L, N_EXPERTS), 0.01),
        'w_gate': nrm(ks[20], (L, N_EXPERTS, D_MODEL, D_EXPERT), D_MODEL ** -0.5),
        'w_up': nrm(ks[21], (L, N_EXPERTS, D_MODEL, D_EXPERT), D_MODEL ** -0.5),
        'w_down': nrm(ks[22], (L, N_EXPERTS, D_EXPERT, D_MODEL), D_EXPERT ** -0.5),
        'g_final': gain(ks[23], (D_MODEL,)),
    }


def reference(x, mem, g_mix, w_in, conv_w, a_log, dt_bias, g_delta_out, g_attn_out, w_out,
              g_cross, g_mem, w_q_mem, w_kv_mem, w_o_mem,
              g_moe, w_group, b_group, w_expert, b_expert, w_gate, w_up, w_down, g_final):
    in_dtype = x.dtype
    h = x.astype(jnp.float32)
    B, S, _ = h.shape
    splits = [W_A, 2 * W_A, 3 * W_A, 3 * W_A + 2 * W_BK + W_BV, 3 * W_A + 2 * W_BK + 2 * W_BV,
              3 * W_A + 2 * W_BK + 2 * W_BV + N_HEADS_B]
    for l in range(DEPTH):
        u = rms_norm(h, g_mix[l])
        proj = u @ w_in[l]
        qa, ka, va, qkv_d, z_d, b_d, a_d = jnp.split(proj, splits, axis=-1)
        attn = dilated_attention(qa.reshape(B, S, N_HEADS_A, HEAD_DIM),
                                 ka.reshape(B, S, N_HEADS_A, HEAD_DIM),
                                 va.reshape(B, S, N_HEADS_A, HEAD_DIM))
        attn = rms_norm(attn.reshape(B, S, W_A), g_attn_out[l])
        delta = gated_deltanet(qkv_d, z_d, b_d, a_d, conv_w[l], a_log[l], dt_bias[l], g_delta_out[l])
        h = h + jnp.concatenate([attn, delta], axis=-1) @ w_out[l]
        mem_n = rms_norm(mem, g_mem[l])
        h = h + memory_cross_attention(rms_norm(h, g_cross[l]), mem_n, w_q_mem[l], w_kv_mem[l], w_o_mem[l])
        h = h + hierarchical_moe(rms_norm(h, g_moe[l]), w_group[l], b_group[l], w_expert[l], b_expert[l],
                                 w_gate[l], w_up[l], w_down[l])
    return rms_norm(h, g_final).astype(in_dtype)
```

```python
import numpy as np
import concourse.bass as bass
import concourse.mybir as mybir
from concourse.bass_utils import run_bass_kernel_spmd
from contextlib import ExitStack

F32 = mybir.dt.float32
BF16 = mybir.dt.bfloat16
AF = mybir.ActivationFunctionType
ALU = mybir.AluOpType

S_FULL = 16384
D = 2048
NCORE = 8
EPS = 1e-6

EPOCH = 24000
NDMA = 16
ENGS = ['pe', 'dve', 'act', 'pool', 'sp']
SAME_ENGINE_WAIT = True
POOL_ENG = 'pool'


def sl(start, n, step=1):
    return slice(start, start + (n - 1) * step + 1, step)


class _Call:
    __slots__ = ('name', 'a', 'k')

    def __init__(self, name, a, k):
        self.name, self.a, self.k = name, a, k

    def run(self, e):
        return getattr(e, self.name)(*self.a, **self.k)


class _Rec:
    def __getattr__(self, name):
        return lambda *a, **k: _Call(name, a, k)


_REC = _Rec()


class Buf:
    __slots__ = ('name', 'w', 'r', 'excl')

    def __init__(self, name='', excl=False):
        self.name = name
        self.w = None
        self.r = {}
        self.excl = excl


class Sched:
    def __init__(self, nc, stack):
        self.nc = nc
        self.stack = stack
        self.ops = {e: [] for e in ENGS}
        self.cnt = {e: 0 for e in ENGS}
        self.sems = {e: [] for e in ENGS}
        self.waited = {e: {} for e in ENGS}
        self.dma_sems = [nc.alloc_semaphore('dq%d' % i) for i in range(2 * NDMA)]
        self.dma_uses = [0] * (2 * NDMA)
        self.dma_i = {'hw': 0, 'sw': 0}
        self.coll_sems = []
        self.dma_sems_extra = {}
        self.n_alloc = 0

    def sb(self, shape, dtype, name=None):
        self.n_alloc += 1
        name = 's_' + (name or ('sb%d' % self.n_alloc))
        return self.stack.enter_context(self.nc.sbuf_tensor(name, list(shape), dtype))

    def ps(self, shape, dtype, name=None):
        self.n_alloc += 1
        name = name or ('ps%d' % self.n_alloc)
        return self.stack.enter_context(self.nc.psum_tensor(name, list(shape), dtype))

    def _semval(self, eng, k):
        ep = (k - 1) // EPOCH
        while len(self.sems[eng]) <= ep:
            self.sems[eng].append(self.nc.alloc_semaphore('s_%s_%d' % (eng, len(self.sems[eng]))))
        return self.sems[eng][ep], (k - 1) % EPOCH + 1

    def _collect(self, reads, writes, eng=None):
        deps = []
        for b in reads:
            if b.w is not None:
                deps.append(b.w)
            if b.excl:
                deps.extend(t for t in b.r.values() if not (t[0] == 'eng' and t[1] == eng))
        for b in writes:
            if b.w is not None:
                deps.append(b.w)
            deps.extend(b.r.values())
        return deps

    def _waits(self, eng, deps):
        best = {}
        for t in deps:
            key = (t[0], t[1])
            if key not in best or best[key] < t[2]:
                best[key] = t[2]
        out = []
        wd = self.waited[eng]
        for key, v in best.items():
            if key[0] == 'eng' and key[1] == eng and (eng == 'pe' or not SAME_ENGINE_WAIT):
                continue
            if wd.get(key, 0) >= v:
                continue
            wd[key] = v
            if key[0] == 'eng':
                out.append(self._semval(key[1], v))
            else:
                out.append((self.dma_sems_extra[key[1]] if key[1] >= 1000 else self.dma_sems[key[1]], v))
        return out

    def _mark(self, tok, reads, writes):
        for b in reads:
            b.r[(tok[0], tok[1])] = tok
        for b in writes:
            b.w = tok
            b.r = {}

    def op(self, eng, fn, reads=(), writes=()):
        waits = self._waits(eng, self._collect(reads, writes, eng))
        k = self.cnt[eng] + 1
        self.cnt[eng] = k
        sem, _ = self._semval(eng, k)

        call = fn(_REC)

        def emit(e, call=call, waits=waits, sem=sem):
            for (s, v) in waits:
                e.wait_ge(s, v)
            call.run(e).then_inc(sem, 1)
        self.ops[eng].append(emit)
        self._mark(('eng', eng, k), reads, writes)

    def dma(self, q, fn, reads=(), writes=()):
        kind = 'sw' if q == 'pool' else 'hw'
        i = self.dma_i[kind] + (NDMA if kind == 'sw' else 0)
        self.dma_i[kind] = (self.dma_i[kind] + 1) % NDMA
        deps = self._collect(reads, writes)
        if self.dma_uses[i] > 0:
            deps.append(('dma', i, 16 * self.dma_uses[i]))
        waits = self._waits(q, deps)
        self.dma_uses[i] += 1
        v = 16 * self.dma_uses[i]
        sem = self.dma_sems[i]

        call = fn(_REC)

        def emit(e, call=call, waits=waits, sem=sem):
            for (s, vv) in waits:
                e.wait_ge(s, vv)
            call.run(e).then_inc(sem, 16)
        self.ops[q].append(emit)
        self._mark(('dma', i, v), reads, writes)

    def coll(self, fn, reads=(), writes=()):
        sem = self.nc.alloc_semaphore('coll%d' % len(self.coll_sems))
        self.coll_sems.append(sem)
        idx = 1000 + len(self.coll_sems)
        self.dma_sems_extra[idx] = sem
        waits = self._waits('pool', self._collect(reads, writes))
        call = fn(_REC)

        def emit(e, call=call, waits=waits, sem=sem):
            for (s, vv) in waits:
                e.wait_ge(s, vv)
            call.run(e).then_inc(sem, 16)
        self.ops['pool'].append(emit)
        self._mark(('dma', idx, 16), reads, writes)

    def finish(self, q='sp'):
        deps = [('dma', i, 16 * u) for i, u in enumerate(self.dma_uses) if u > 0]
        deps += [('dma', i, 16) for i in self.dma_sems_extra]
        for e in ENGS:
            if self.cnt[e] > 0:
                deps.append(('eng', e, self.cnt[e]))
        waits = self._waits(q, deps)

        def emit(e, waits=waits):
            for (s, vv) in waits:
                e.wait_ge(s, vv)
        self.ops[q].append(emit)

    def build(self):
        self.finish()
        ops = self.ops
        with self.nc.Block() as block:
            @block.sync
            def _(e):
                for f in ops['sp']:
                    f(e)

            @block.scalar
            def _(e):
                for f in ops['act']:
                    f(e)

            @block.vector
            def _(e):
                for f in ops['dve']:
                    f(e)

            @block.gpsimd
            def _(e):
                for f in ops['pool']:
                    f(e)

            @block.tensor
            def _(e):
                for f in ops['pe']:
                    f(e)


class NormT:
    def __init__(self, S, ident_f, b_ident, pT, b_pT):
        self.S = S
        self.ident = ident_f
        self.b_ident = b_ident
        self.pT = pT
        self.b_pT = b_pT
        self.hs = S.sb([128, D], F32, 'nt_hs')
        self.b_hs = Buf()
        self.ss = S.sb([128, 1], F32, 'nt_ss')
        self.b_ss = Buf()
        self.rstd = S.sb([128, 1], F32, 'nt_rstd')
        self.b_rstd = Buf()
        self.flip = 0

    def stats(self, src, b_src):
        S = self.S
        hs, ss, rstd = self.hs, self.ss, self.rstd
        S.op('act', lambda e: e.activation(out=hs[:], in_=src, func=AF.Square, accum_out=ss[:]),
             reads=[b_src], writes=[self.b_hs, self.b_ss])
        S.op('dve', lambda e: e.tensor_scalar(out=rstd[:], in0=ss[:], scalar1=1.0 / D, scalar2=EPS,
                                              op0=ALU.mult, op1=ALU.add), reads=[self.b_ss], writes=[self.b_rstd])
        S.op('act', lambda e: e.activation(out=rstd[:], in_=rstd[:], func=AF.Sqrt),
             reads=[self.b_rstd], writes=[self.b_rstd])
        S.op('dve', lambda e: e.reciprocal(out=rstd[:], in_=rstd[:]), reads=[self.b_rstd], writes=[self.b_rstd])
        S.op('act', lambda e: e.activation(out=hs[:], in_=src, func=AF.Copy, scale=rstd[:]),
             reads=[b_src, self.b_rstd], writes=[self.b_hs])

    def run(self, src, b_src, gain, b_gain, dst, b_dst, dst32=None, b_dst32=None):
        S = self.S
        self.stats(src, b_src)
        hs = self.hs
        for g in range(4):
            p = self.flip
            self.flip ^= 1
            pT, b_pT = self.pT[p], self.b_pT[p]
            for j in range(4):
                k = g * 4 + j
                S.op('pe', lambda e, k=k, j=j, pT=pT: e.transpose(out=pT[:, j * 128:(j + 1) * 128],
                                                                   in_=hs[:, k * 128:(k + 1) * 128],
                                                                   identity=self.ident[:]),
                     reads=[self.b_hs, self.b_ident], writes=[b_pT])
            for j in range(4):
                k = g * 4 + j
                if j % 2 == 0:
                    S.op('act', lambda e, k=k, j=j, pT=pT: e.activation(out=dst[:, k, :], in_=pT[:, j * 128:(j + 1) * 128],
                                                                         func=AF.Copy, scale=gain[:, k:k + 1]),
                         reads=[b_pT, b_gain], writes=[b_dst])
                else:
                    S.op('dve', lambda e, k=k, j=j, pT=pT: e.tensor_scalar(out=dst[:, k, :], in0=pT[:, j * 128:(j + 1) * 128],
                                                                            scalar1=gain[:, k:k + 1], scalar2=None,
                                                                            op0=ALU.mult),
                         reads=[b_pT, b_gain], writes=[b_dst])
                if dst32 is not None:
                    S.op('dve', lambda e, k=k, j=j, pT=pT: e.tensor_scalar(out=dst32[:, k, :], in0=pT[:, j * 128:(j + 1) * 128],
                                                                            scalar1=gain[:, k:k + 1], scalar2=None,
                                                                            op0=ALU.mult),
                         reads=[b_pT, b_gain], writes=[b_dst32])


TB = 512
NTB = 4
TOK_B = 2048
N_EXP = 64
BIG = 1.0e4


def build_phase_b(nc, mix_dtype=F32, n_blocks=TOK_B // TB, n_exp=N_EXP):
    dram = {}

    def din(name, shape, dt=F32):
        dram[name] = nc.dram_tensor(name, list(shape), dt, kind='ExternalInput').ap()
        return dram[name]

    x = din('xs', [TOK_B, D])
    mixT = din('mixT', [16, 128, TOK_B], mix_dtype)
    w_out = din('w_out', [D, D])
    w_q = din('w_q', [D, 512])
    w_kv = din('w_kv', [D, 1024])
    w_o = din('w_o', [512, D])
    mem = din('mem', [256, D])
    g_attn = din('g_attn', [128, 8])
    g_cross = din('g_cross', [128, 16])
    g_mem = din('g_mem', [128, 16])
    g_moe = din('g_moe', [128, 16])
    g_final = din('g_final', [D])
    w_r = din('w_r', [D, 72])
    b_r = din('b_r', [72])
    ne_d = max(n_exp, 1)
    w_gate = din('w_gate', [ne_d, D, 512])
    w_up = din('w_up', [ne_d, D, 512])
    w_down = din('w_down', [ne_d, 512, D])
    ident = din('ident', [128, 128])
    y = nc.dram_tensor('y', [TOK_B, D], F32, kind='ExternalOutput').ap()

    with ExitStack() as st:
        S = Sched(nc, st)
        ident_f = S.sb([128, 128], F32, 'ident_f'); b_ident = Buf()
        ones_bf = S.sb([128, 128], BF16, 'ones_bf'); b_ones = Buf()
        gA = S.sb([128, 8], F32, 'gA'); gC = S.sb([128, 16], F32, 'gC')
        gM = S.sb([128, 16], F32, 'gM'); gE = S.sb([128, 16], F32, 'gE')
        b_g = Buf()
        gF = S.sb([128, D], F32, 'gF'); b_gF = Buf()
        bR = S.sb([128, 72], F32, 'bR'); b_bR = Buf()
        wR = S.sb([128, 16, 72], F32, 'wR'); b_wR = Buf()
        S.dma('sp', lambda e: e.dma_start(out=ident_f[:], in_=ident), writes=[b_ident])
        S.dma('sp', lambda e: e.dma_start(out=gA[:], in_=g_attn), writes=[b_g])
        S.dma('sp', lambda e: e.dma_start(out=gC[:], in_=g_cross), writes=[b_g])
        S.dma('sp', lambda e: e.dma_start(out=gM[:], in_=g_mem), writes=[b_g])
        S.dma('sp', lambda e: e.dma_start(out=gE[:], in_=g_moe), writes=[b_g])
        S.dma('sp', lambda e: e.dma_start(out=gF[:], in_=g_final.partition_broadcast(128)), writes=[b_gF])
        S.dma('sp', lambda e: e.dma_start(out=bR[:], in_=b_r.partition_broadcast(128)), writes=[b_bR])
        S.dma('sp', lambda e: e.dma_start(out=wR[:], in_=w_r.rearrange('(kc p) n -> p kc n', p=128)), writes=[b_wR])
        S.op('dve', lambda e: e.memset(ones_bf[:], 1.0), writes=[b_ones])

        banks = [S.ps([128, 512], F32, 'bank%d' % i) for i in range(8)]
        b_bank = [Buf(excl=True) for _ in range(8)]
        pT, b_pT = banks[0:2], b_bank[0:2]
        pG, b_pG = banks[2:4], b_bank[2:4]
        pU, b_pU = banks[4:6], b_bank[4:6]
        pY, b_pY = banks[6:8], b_bank[6:8]

        NT = NormT(S, ident_f, b_ident, pT, b_pT)

        h = S.sb([128, NTB, D], F32, 'h'); b_h = [Buf() for _ in range(NTB)]
        actT = S.sb([128, 16, TB], BF16, 'actT'); b_actT = Buf()
        hnT32 = S.sb([128, 16, 128], F32, 'hnT32'); b_hnT32 = Buf()
        NA, NB = 3, 2
        poolA = [S.sb([128, 16, 512], BF16, 'poolA%d' % i) for i in range(NA)]
        b_poolA = [Buf() for _ in range(NA)]
        poolB = [S.sb([128, 4, D], BF16, 'poolB%d' % i) for i in range(NB)]
        b_poolB = [Buf() for _ in range(NB)]
        cntA = [0]
        cntB = [0]

        def loadA(src):
            i = cntA[0] % NA
            cntA[0] += 1
            S.dma('pool', lambda e, i=i: e.dma_start(out=poolA[i][:], in_=src.rearrange('(kc p) n -> p kc n', p=128)),
                  writes=[b_poolA[i]])
            return poolA[i], b_poolA[i]

        def loadB(src):
            i = cntB[0] % NB
            cntB[0] += 1
            S.dma('pool', lambda e, i=i: e.dma_start(out=poolB[i][:], in_=src.rearrange('(kc p) n -> p kc n', p=128)),
                  writes=[b_poolB[i]])
            return poolB[i], b_poolB[i]

        KmT = S.sb([128, 4, 256], BF16, 'KmT'); b_KmT = Buf()
        Vm = S.sb([128, 2, 512], BF16, 'Vm'); b_Vm = Buf()
        qT = S.sb([128, 4, TB], BF16, 'qT'); b_qT = Buf()
        PT = S.sb([128, 2, TB], BF16, 'PT'); b_PT = Buf()
        oT = S.sb([128, 4, TB], BF16, 'oT'); b_oT = Buf()
        rden = S.sb([128, TB], F32, 'rden'); b_rden = Buf()
        sq = S.sb([128, TB], BF16, 'sq'); b_sq = Buf()
        rsA = S.sb([128, TB], F32, 'rsA'); b_rsA = Buf()
        hid = [S.sb([128, 4, TB], BF16, 'hid%d' % i) for i in range(2)]
        b_hid = [Buf(), Buf()]
        sg = [S.sb([128, TB], F32, 'sg%d' % i) for i in range(2)]
        b_sg = [Buf(), Buf()]
        lg = S.sb([128, 72], F32, 'lg'); b_lg = Buf()
        rt = S.sb([128, 16], F32, 'rt'); b_rt = Buf()
        ohg = S.sb([128, 8], F32, 'ohg'); b_ohg = Buf()
        lm = S.sb([128, 64], F32, 'lm'); b_lm = Buf()
        m8 = S.sb([128, 8], F32, 'm8'); b_m8 = Buf()
        mk = S.sb([128, 64], F32, 'mk'); b_mk = Buf()
        wts = S.sb([128, NTB, 64], F32, 'wts'); b_wts = Buf()

        memt = h
        for t in range(2):
            S.dma('sp', lambda e, t=t: e.dma_start(out=h[:, t, :], in_=mem[t * 128:(t + 1) * 128, :]), writes=[b_h[t]])
            NT.run(h[:, t, :], b_h[t], gM, b_g, actT[:, :, t * 128:(t + 1) * 128], b_actT)
        for half in range(2):
            W, b_W = loadA(w_kv[:, half * 512:(half + 1) * 512])
            if half == 0:
                for hd in range(4):
                    for k in range(16):
                        S.op('pe', lambda e, k=k, hd=hd, W=W: e.matmul(pG[hd % 2][:, 0:256],
                                                                         lhsT=W[:, k, hd * 128:(hd + 1) * 128], rhs=actT[:, k, 0:256],
                                                                         start=(k == 0), stop=(k == 15)),
                             reads=[b_W, b_actT], writes=[b_pG[hd % 2]])
                    S.op('act', lambda e, hd=hd: e.activation(out=KmT[:, hd, :], in_=pG[hd % 2][:, 0:256], func=AF.Copy),
                         reads=[b_pG[hd % 2]], writes=[b_KmT])
            else:
                for mt in range(2):
                    for k in range(16):
                        S.op('pe', lambda e, k=k, mt=mt, W=W: e.matmul(pU[mt][:], lhsT=actT[:, k, mt * 128:(mt + 1) * 128],
                                                                         rhs=W[:, k, :], start=(k == 0), stop=(k == 15)),
                             reads=[b_W, b_actT], writes=[b_pU[mt]])
                    S.op('act', lambda e, mt=mt: e.activation(out=Vm[:, mt, :], in_=pU[mt][:], func=AF.Copy),
                         reads=[b_pU[mt]], writes=[b_Vm])

        scale_attn = 128.0 ** -0.5
        for blk in range(n_blocks):
            t0 = blk * TB
            for t in range(NTB):
                S.dma('sp', lambda e, t=t: e.dma_start(out=h[:, t, :], in_=x[t0 + t * 128:t0 + (t + 1) * 128, :]),
                      writes=[b_h[t]])
            S.dma('pool', lambda e: e.dma_start(out=actT[:], in_=mixT[:, :, t0:t0 + TB].rearrange('c p t -> p c t')),
                  writes=[b_actT])
            for c in range(8):
                S.op('dve', lambda e, c=c: e.tensor_tensor(out=sq[:], in0=actT[:, c, :], in1=actT[:, c, :], op=ALU.mult),
                     reads=[b_actT], writes=[b_sq])
                S.op('pe', lambda e, c=c: e.matmul(pG[0][:], lhsT=ones_bf[:], rhs=sq[:], start=(c == 0), stop=(c == 7)),
                     reads=[b_ones, b_sq], writes=[b_pG[0]])
            S.op('dve', lambda e: e.tensor_scalar(out=rsA[:], in0=pG[0][:], scalar1=1.0 / 1024, scalar2=EPS,
                                                  op0=ALU.mult, op1=ALU.add), reads=[b_pG[0]], writes=[b_rsA])
            S.op('act', lambda e: e.activation(out=rsA[:], in_=rsA[:], func=AF.Sqrt), reads=[b_rsA], writes=[b_rsA])
            S.op('dve', lambda e: e.reciprocal(out=rsA[:], in_=rsA[:]), reads=[b_rsA], writes=[b_rsA])
            for c in range(8):
                S.op('dve', lambda e, c=c: e.scalar_tensor_tensor(out=actT[:, c, :], in0=actT[:, c, :], scalar=gA[:, c:c + 1],
                                                                   in1=rsA[:], op0=ALU.mult, op1=ALU.mult),
                     reads=[b_actT, b_g, b_rsA], writes=[b_actT])
            for n in range(4):
                W, b_W = loadA(w_out[:, n * 512:(n + 1) * 512])
                for t in range(NTB):
                    p = (n * NTB + t) % 2
                    for k in range(16):
                        S.op('pe', lambda e, k=k, t=t, p=p, W=W: e.matmul(pY[p][:], lhsT=actT[:, k, t * 128:(t + 1) * 128],
                                                                           rhs=W[:, k, :], start=(k == 0), stop=(k == 15)),
                             reads=[b_actT, b_W], writes=[b_pY[p]])
                    S.op('dve', lambda e, t=t, n=n, p=p: e.tensor_tensor(out=h[:, t, n * 512:(n + 1) * 512], in0=pY[p][:],
                                                                          in1=h[:, t, n * 512:(n + 1) * 512], op=ALU.add),
                         reads=[b_pY[p], b_h[t]], writes=[b_h[t]])
            for t in range(NTB):
                NT.run(h[:, t, :], b_h[t], gC, b_g, actT[:, :, t * 128:(t + 1) * 128], b_actT)
            W, b_W = loadA(w_q)
            for hd in range(4):
                p = hd % 2
                for k in range(16):
                    S.op('pe', lambda e, k=k, hd=hd, p=p, W=W: e.matmul(pG[p][:], lhsT=W[:, k, hd * 128:(hd + 1) * 128],
                                                                         rhs=actT[:, k, :], start=(k == 0), stop=(k == 15)),
                         reads=[b_W, b_actT], writes=[b_pG[p]])
                S.op('act', lambda e, hd=hd, p=p: e.activation(out=qT[:, hd, :], in_=pG[p][:], func=AF.Copy),
                     reads=[b_pG[p]], writes=[b_qT])
            for hd in range(4):
                for mt in range(2):
                    S.op('pe', lambda e, hd=hd, mt=mt: e.matmul(pU[mt][:], lhsT=KmT[:, hd, mt * 128:(mt + 1) * 128],
                                                                 rhs=qT[:, hd, :], start=True, stop=True),
                         reads=[b_KmT, b_qT], writes=[b_pU[mt]])
                    S.op('act', lambda e, mt=mt: e.activation(out=PT[:, mt, :], in_=pU[mt][:], func=AF.Exp, scale=scale_attn),
                         reads=[b_pU[mt]], writes=[b_PT])
                for mt in range(2):
                    S.op('pe', lambda e, hd=hd, mt=mt: e.matmul(pG[0][:], lhsT=Vm[:, mt, hd * 128:(hd + 1) * 128],
                                                                 rhs=PT[:, mt, :], start=(mt == 0), stop=(mt == 1)),
                         reads=[b_Vm, b_PT], writes=[b_pG[0]])
                for mt in range(2):
                    S.op('pe', lambda e, mt=mt: e.matmul(pG[1][:], lhsT=ones_bf[:], rhs=PT[:, mt, :],
                                                          start=(mt == 0), stop=(mt == 1)),
                         reads=[b_ones, b_PT], writes=[b_pG[1]])
                S.op('dve', lambda e: e.reciprocal(out=rden[:], in_=pG[1][:]), reads=[b_pG[1]], writes=[b_rden])
                S.op('dve', lambda e, hd=hd: e.tensor_tensor(out=oT[:, hd, :], in0=pG[0][:], in1=rden[:], op=ALU.mult),
                     reads=[b_pG[0], b_rden], writes=[b_oT])
            Wo, b_Wo = loadB(w_o)
            for t in range(NTB):
                for n in range(4):
                    p = (t * 4 + n) % 2
                    for hd in range(4):
                        S.op('pe', lambda e, hd=hd, t=t, n=n, p=p, Wo=Wo: e.matmul(pY[p][:], lhsT=oT[:, hd, t * 128:(t + 1) * 128],
                                                                                   rhs=Wo[:, hd, n * 512:(n + 1) * 512],
                                                                                   start=(hd == 0), stop=(hd == 3)),
                             reads=[b_oT, b_Wo], writes=[b_pY[p]])
                    S.op('dve', lambda e, t=t, n=n, p=p: e.tensor_tensor(out=h[:, t, n * 512:(n + 1) * 512], in0=pY[p][:],
                                                                          in1=h[:, t, n * 512:(n + 1) * 512], op=ALU.add),
                         reads=[b_pY[p], b_h[t]], writes=[b_h[t]])
            for t in range(NTB):
                NT.run(h[:, t, :], b_h[t], gE, b_g, actT[:, :, t * 128:(t + 1) * 128], b_actT, hnT32, b_hnT32)
                for k in range(16):
                    S.op('pe', lambda e, k=k: e.matmul(pU[0][:, 0:72], lhsT=hnT32[:, k, :], rhs=wR[:, k, :],
                                                        start=(k == 0), stop=(k == 15)),
                         reads=[b_hnT32, b_wR], writes=[b_pU[0]])
                S.op('dve', lambda e: e.tensor_tensor(out=lg[:], in0=pU[0][:, 0:72], in1=bR[:], op=ALU.add),
                     reads=[b_pU[0], b_bR], writes=[b_lg])
                S.op('dve', lambda e: e.reduce_max(out=rt[:, 0:1], in_=lg[:, 0:8], axis=mybir.AxisListType.X),
                     reads=[b_lg], writes=[b_rt])
                S.op('dve', lambda e: e.tensor_scalar(out=ohg[:], in0=lg[:, 0:8], scalar1=rt[:, 0:1], scalar2=None,
                                                      op0=ALU.is_equal), reads=[b_lg, b_rt], writes=[b_ohg])
                S.op('dve', lambda e: e.tensor_scalar(out=rt[:, 1:2], in0=rt[:, 0:1], scalar1=-1.0, scalar2=None, op0=ALU.mult),
                     reads=[b_rt], writes=[b_rt])
                S.op('act', lambda e: e.activation(out=m8[:], in_=lg[:, 0:8], func=AF.Exp, bias=rt[:, 1:2], accum_out=rt[:, 2:3]),
                     reads=[b_lg, b_rt], writes=[b_m8, b_rt])
                S.op('dve', lambda e: e.reciprocal(out=rt[:, 3:4], in_=rt[:, 2:3]), reads=[b_rt], writes=[b_rt])
                S.op('dve', lambda e: e.tensor_scalar(out=ohg[:], in0=ohg[:], scalar1=BIG, scalar2=-BIG,
                                                      op0=ALU.mult, op1=ALU.add), reads=[b_ohg], writes=[b_ohg])
                for g in range(8):
                    S.op('dve', lambda e, g=g: e.tensor_scalar(out=lm[:, g * 8:(g + 1) * 8], in0=lg[:, 8 + g * 8:16 + g * 8],
                                                               scalar1=ohg[:, g:g + 1], scalar2=None, op0=ALU.add),
                         reads=[b_lg, b_ohg], writes=[b_lm])
                S.op('dve', lambda e: e.max(out=m8[:], in_=lm[:]), reads=[b_lm], writes=[b_m8])
                S.op('dve', lambda e: e.tensor_tensor(out=rt[:, 4:5], in0=m8[:, 0:1], in1=m8[:, 1:2], op=ALU.subtract),
                     reads=[b_m8], writes=[b_rt])
                S.op('act', lambda e: e.activation(out=rt[:, 5:6], in_=rt[:, 4:5], func=AF.Sigmoid),
                     reads=[b_rt], writes=[b_rt])
                S.op('dve', lambda e: e.tensor_tensor(out=rt[:, 6:7], in0=rt[:, 5:6], in1=rt[:, 3:4], op=ALU.mult),
                     reads=[b_rt], writes=[b_rt])
                S.op('dve', lambda e: e.tensor_tensor(out=rt[:, 7:8], in0=rt[:, 3:4], in1=rt[:, 6:7], op=ALU.subtract),
                     reads=[b_rt], writes=[b_rt])
                S.op('dve', lambda e: e.tensor_scalar(out=mk[:], in0=lm[:], scalar1=m8[:, 0:1], scalar2=rt[:, 6:7],
                                                      op0=ALU.is_equal, op1=ALU.mult), reads=[b_lm, b_m8, b_rt], writes=[b_mk])
                S.op('dve', lambda e, t=t: e.tensor_scalar(out=wts[:, t, :], in0=lm[:], scalar1=m8[:, 1:2], scalar2=rt[:, 7:8],
                                                           op0=ALU.is_equal, op1=ALU.mult),
                     reads=[b_lm, b_m8, b_rt], writes=[b_wts])
                S.op('dve', lambda e, t=t: e.tensor_tensor(out=wts[:, t, :], in0=wts[:, t, :], in1=mk[:], op=ALU.add),
                     reads=[b_mk, b_wts], writes=[b_wts])
            for ex in range(n_exp):
                Wg, b_Wg = loadA(w_gate[ex])
                Wu, b_Wu = loadA(w_up[ex])
                Wd, b_Wd = loadB(w_down[ex])
                hp = ex % 2
                for fc in range(4):
                    p = fc % 2
                    for k in range(16):
                        S.op('pe', lambda e, k=k, fc=fc, p=p, Wg=Wg: e.matmul(pG[p][:], lhsT=Wg[:, k, fc * 128:(fc + 1) * 128],
                                                                               rhs=actT[:, k, :], start=(k == 0), stop=(k == 15)),
                             reads=[b_Wg, b_actT], writes=[b_pG[p]])
                    for k in range(16):
                        S.op('pe', lambda e, k=k, fc=fc, p=p, Wu=Wu: e.matmul(pU[p][:], lhsT=Wu[:, k, fc * 128:(fc + 1) * 128],
                                                                               rhs=actT[:, k, :], start=(k == 0), stop=(k == 15)),
                             reads=[b_Wu, b_actT], writes=[b_pU[p]])
                    S.op('act', lambda e, p=p: e.activation(out=sg[p][:], in_=pG[p][:], func=AF.Silu),
                         reads=[b_pG[p]], writes=[b_sg[p]])
                    S.op('dve', lambda e, p=p, fc=fc, hp=hp: e.tensor_tensor(out=hid[hp][:, fc, :], in0=pU[p][:], in1=sg[p][:],
                                                                              op=ALU.mult),
                         reads=[b_pU[p], b_sg[p]], writes=[b_hid[hp]])
                for t in range(NTB):
                    for n in range(4):
                        p = (t * 4 + n) % 2
                        for fc in range(4):
                            S.op('pe', lambda e, fc=fc, t=t, n=n, p=p, hp=hp, Wd=Wd: e.matmul(
                                pY[p][:], lhsT=hid[hp][:, fc, t * 128:(t + 1) * 128], rhs=Wd[:, fc, n * 512:(n + 1) * 512],
                                start=(fc == 0), stop=(fc == 3)),
                                 reads=[b_hid[hp], b_Wd], writes=[b_pY[p]])
                        S.op('dve', lambda e, t=t, n=n, p=p, ex=ex: e.scalar_tensor_tensor(
                            out=h[:, t, n * 512:(n + 1) * 512], in0=pY[p][:], scalar=wts[:, t, ex:ex + 1],
                            in1=h[:, t, n * 512:(n + 1) * 512], op0=ALU.mult, op1=ALU.add),
                             reads=[b_pY[p], b_wts, b_h[t]], writes=[b_h[t]])
            for t in range(NTB):
                NT.stats(h[:, t, :], b_h[t])
                S.op('dve', lambda e: e.tensor_tensor(out=NT.hs[:], in0=NT.hs[:], in1=gF[:], op=ALU.mult),
                     reads=[NT.b_hs, b_gF], writes=[NT.b_hs])
                S.dma('sp', lambda e, t=t: e.dma_start(out=y[t0 + t * 128:t0 + (t + 1) * 128, :], in_=NT.hs[:]),
                      reads=[NT.b_hs])
        S.build()
    return nc


def _cols(v, n):
    return np.ascontiguousarray(np.asarray(v, dtype=np.float32).reshape(n, 128).T)


def phase_b_inputs(inp, mixT_full, n_exp=N_EXP):
    x = inp['x'][0]
    shared = {
        'w_out': inp['w_out'][0], 'w_q': inp['w_q_mem'][0], 'w_kv': inp['w_kv_mem'][0], 'w_o': inp['w_o_mem'][0],
        'mem': inp['mem'][0], 'g_attn': _cols(inp['g_attn_out'][0], 8), 'g_cross': _cols(inp['g_cross'][0], 16),
        'g_mem': _cols(inp['g_mem'][0], 16), 'g_moe': _cols(inp['g_moe'][0], 16),
        'g_final': np.ascontiguousarray(inp['g_final']),
        'w_r': np.ascontiguousarray(np.concatenate([inp['w_group'][0], inp['w_expert'][0]], axis=1)),
        'b_r': np.ascontiguousarray(np.concatenate([inp['b_group'][0], inp['b_expert'][0]], axis=0)),
        'w_gate': inp['w_gate'][0][:max(n_exp, 1)], 'w_up': inp['w_up'][0][:max(n_exp, 1)],
        'w_down': inp['w_down'][0][:max(n_exp, 1)],
        'ident': np.eye(128, dtype=np.float32),
    }
    maps = []
    for c in range(NCORE):
        s = slice(c * TOK_B, (c + 1) * TOK_B)
        m = dict(shared)
        m['xs'] = np.ascontiguousarray(x[s])
        m['mixT'] = np.ascontiguousarray(mixT_full[:, :, s])
        maps.append(m)
    return maps


SB = 2048
SUB = 512
NW = 898
NEG = -30000.0
PATTERNS = (1, 4, 16)


def build_phase_a(nc, n_sb=S_FULL // SB, out_dtype=F32, do_attn=True, do_delta=True):
    dram = {}

    def din(name, shape, dt=F32):
        dram[name] = nc.dram_tensor(name, list(shape), dt, kind='ExternalInput').ap()
        return dram[name]

    x = din('x', [S_FULL, D])
    w_sel = din('w_sel', [D, NW])
    g_mix = din('g_mix', [128, 16])
    conv_w = din('conv_w', [128, 12])
    a_log = din('a_log', [128, 1])
    dt_b = din('dt_b', [128, 1])
    g_do = din('g_do', [128, 1])
    abias = din('abias', [128, 3, 256])
    lstrict = din('lstrict', [128, 512])
    uincl = din('uincl', [128, 512])
    ident4_d = din('ident4', [128, 512])
    attnT = nc.dram_tensor('attnT', [128, S_FULL], out_dtype, kind='ExternalOutput').ap()
    deltaT = nc.dram_tensor('deltaT', [128, S_FULL], out_dtype, kind='ExternalOutput').ap()

    with ExitStack() as st:
        S = Sched(nc, st)
        b_c = Buf()
        ident4 = S.sb([128, 512], F32, 'ident4')
        ident_f = ident4[:, 0:128]
        Ls4 = S.sb([128, 512], F32, 'Ls4')
        Ui4 = S.sb([128, 512], F32, 'Ui4')
        ones_f = S.sb([128, 128], F32, 'ones_f')
        ones_bf = S.sb([128, 128], BF16, 'ones_bf')
        gMix = S.sb([128, 16], F32, 'gMix')
        cw = S.sb([128, 12], F32, 'cw')
        alog = S.sb([128, 1], F32, 'alog')
        dtb = S.sb([128, 1], F32, 'dtb')
        gdo = S.sb([128, 1], F32, 'gdo')
        negA = S.sb([128, 1], F32, 'negA')
        ab = S.sb([128, 3, 256], F32, 'ab')
        Wsel = S.sb([128, 16, NW], BF16, 'Wsel'); b_W = Buf()
        for dst, src in ((ident4, ident4_d), (Ls4, lstrict), (Ui4, uincl), (gMix, g_mix), (cw, conv_w), (alog, a_log),
                         (dtb, dt_b), (gdo, g_do), (ab, abias)):
            S.dma('sp', lambda e, dst=dst, src=src: e.dma_start(out=dst[:], in_=src), writes=[b_c])
        S.dma('pool', lambda e: e.dma_start(out=Wsel[:], in_=w_sel.rearrange('(kc p) n -> p kc n', p=128)), writes=[b_W])
        S.op('dve', lambda e: e.memset(ones_f[:], 1.0), writes=[b_c])
        S.op('dve', lambda e: e.memset(ones_bf[:], 1.0), writes=[b_c])
        S.op('act', lambda e: e.activation(out=negA[:], in_=alog[:], func=AF.Exp), reads=[b_c], writes=[b_c])
        S.op('dve', lambda e: e.tensor_scalar(out=negA[:], in0=negA[:], scalar1=-1.0, scalar2=None, op0=ALU.mult),
             reads=[b_c], writes=[b_c])

        banks = [S.ps([128, 512], F32, 'bank%d' % i) for i in range(8)]
        b_bank = [Buf(excl=True) for _ in range(8)]
        NT = NormT(S, ident_f, b_c, banks[0:2], b_bank[0:2])
        pO, b_pO = banks[2], b_bank[2]
        pS, b_pS = banks[3], b_bank[3]
        gen_i = [0]

        def gbank():
            i = 4 + gen_i[0] % 4
            gen_i[0] += 1
            return banks[i], b_bank[i]

        xt = S.sb([128, D], F32, 'xt'); b_xt = Buf()
        uT = S.sb([128, 16, SUB], BF16, 'uT'); b_uT = Buf()
        qaT = S.sb([128, SB], BF16, 'qaT'); b_qaT = Buf()
        kaT = [S.sb([128, SB], BF16, 'kaT%d' % i) for i in range(2)]; b_kaT = [Buf(), Buf()]
        vaT = S.sb([128, SB], F32, 'vaT'); b_vaT = Buf()
        Vt = [[S.sb([128, 16, 128], BF16, 'Vt%d_%d' % (p, i)) for i in range(2)] for p in range(3)]
        b_Vt = [[Buf(), Buf()] for _ in range(3)]
        acc = S.sb([128, 2, SB], F32, 'acc'); b_acc = Buf()
        atmp = [S.sb([128, 256], F32, 'atmp%d' % i) for i in range(2)]; b_atmp = [Buf(), Buf()]
        aPT = [S.sb([128, 256], BF16, 'aPT%d' % i) for i in range(2)]; b_aPT = [Buf(), Buf()]
        cbuf = [S.sb([128, SUB + 3], F32, 'cbuf%d' % i) for i in range(3)]; b_cbuf = [Buf() for _ in range(3)]
        ctmp = S.sb([128, SUB], F32, 'ctmp'); b_ctmp = Buf()
        QKV = [S.sb([128, SUB], F32, 'qkv%d' % i) for i in range(3)]; b_QKV = [Buf() for _ in range(3)]
        zs = S.sb([128, SUB], F32, 'zs'); b_zs = Buf()
        ba = S.sb([128, 4, 2], F32, 'ba'); b_ba = Buf()
        sc = S.sb([128, 8, 4], F32, 'sc'); b_sc = Buf()
        names = ['diagG', 'tA', 'tB', 'expR', 'Kbe', 'Kdec', 'Vb', 'B0', 'B1', 'BT0', 'BT1', 'TT', 'qkT', 'qdT',
                 'u', 'wT', 'sqo', 'rs', 'dout']
        T = {}
        bT = {}
        for nm in names:
            T[nm] = S.sb([128, SUB], F32, 'd_' + nm)
            bT[nm] = Buf()
        vnew = S.sb([128, 128], F32, 'vnew'); b_vnew = Buf()
        Sst = S.sb([128, 128], F32, 'Sst'); b_Sst = Buf()
        S.op('dve', lambda e: e.memset(Sst[:], 0.0), writes=[b_Sst])
        for i in range(3):
            S.op('dve', lambda e, i=i: e.memset(cbuf[i][:, 0:3], 0.0), writes=[b_cbuf[i]])


        def v4(t):
            return t[:].rearrange('p (a b) -> p a b', a=4)

        q_scale = 128.0 ** -0.5

        for sb in range(n_sb):
            par = sb % 2
            for j in range(SB // SUB):
                tok0 = sb * SB + j * SUB
                c0 = j * SUB
                for t in range(4):
                    S.dma('sp', lambda e, t=t: e.dma_start(out=xt[:], in_=x[tok0 + t * 128:tok0 + (t + 1) * 128, :]),
                          writes=[b_xt])
                    NT.run(xt[:], b_xt, gMix, b_c, uT[:, :, t * 128:(t + 1) * 128], b_uT)
                for t in range(4):
                    for k in range(16):
                        S.op('pe', lambda e, k=k, t=t: e.matmul(pS[:, t * 2:t * 2 + 2], lhsT=uT[:, k, t * 128:(t + 1) * 128],
                                                                 rhs=Wsel[:, k, 896:898], start=(k == 0), stop=(k == 15)),
                             reads=[b_uT, b_W], writes=[b_pS])
                S.op('dve', lambda e: e.tensor_copy(out=ba[:].rearrange('p a b -> p (a b)'), in_=pS[:, 0:8]),
                     reads=[b_pS], writes=[b_ba])
                for o in range(7):
                    if (o < 3 and not do_attn) or (o >= 3 and not do_delta):
                        continue
                    pb, b_pb = gbank()
                    for k in range(16):
                        S.op('pe', lambda e, k=k, o=o, pb=pb: e.matmul(pb[:], lhsT=Wsel[:, k, o * 128:(o + 1) * 128],
                                                                       rhs=uT[:, k, :], start=(k == 0), stop=(k == 15)),
                             reads=[b_W, b_uT], writes=[b_pb])
                    if o == 0:
                        S.op('act', lambda e, pb=pb: e.activation(out=qaT[:, c0:c0 + SUB], in_=pb[:], func=AF.Copy,
                                                                  scale=q_scale), reads=[b_pb], writes=[b_qaT])
                    elif o == 1:
                        S.op('act', lambda e, pb=pb: e.activation(out=kaT[par][:, c0:c0 + SUB], in_=pb[:], func=AF.Copy),
                             reads=[b_pb], writes=[b_kaT[par]])
                    elif o == 2:
                        S.op('act', lambda e, pb=pb: e.activation(out=vaT[:, c0:c0 + SUB], in_=pb[:], func=AF.Copy),
                             reads=[b_pb], writes=[b_vaT])
                    elif o < 6:
                        i = o - 3
                        S.op('act', lambda e, pb=pb, i=i: e.activation(out=cbuf[i][:, 3:SUB + 3], in_=pb[:], func=AF.Copy),
                             reads=[b_pb], writes=[b_cbuf[i]])
                    else:
                        S.op('act', lambda e, pb=pb: e.activation(out=zs[:], in_=pb[:], func=AF.Silu),
                             reads=[b_pb], writes=[b_zs])
                if not do_delta:
                    continue
                for i in range(3):
                    cb = cbuf[i]
                    S.op('dve', lambda e, cb=cb, i=i: e.tensor_scalar(out=ctmp[:], in0=cb[:, 3:SUB + 3],
                                                                      scalar1=cw[:, i * 4 + 3:i * 4 + 4], scalar2=None,
                                                                      op0=ALU.mult),
                         reads=[b_cbuf[i], b_c], writes=[b_ctmp])
                    for jj in (2, 1, 0):
                        S.op('dve', lambda e, cb=cb, i=i, jj=jj: e.scalar_tensor_tensor(
                            out=ctmp[:], in0=cb[:, jj:jj + SUB], scalar=cw[:, i * 4 + jj:i * 4 + jj + 1], in1=ctmp[:],
                            op0=ALU.mult, op1=ALU.add), reads=[b_cbuf[i], b_c, b_ctmp], writes=[b_ctmp])
                    S.op('dve', lambda e, cb=cb: e.tensor_copy(out=cb[:, 0:3], in_=cb[:, SUB:SUB + 3]),
                         reads=[b_cbuf[i]], writes=[b_cbuf[i]])
                    S.op('act', lambda e, i=i: e.activation(out=QKV[i][:], in_=ctmp[:], func=AF.Silu),
                         reads=[b_ctmp], writes=[b_QKV[i]])
                    if i < 2:
                        S.op('act', lambda e, i=i: e.activation(out=T['sqo'][:], in_=QKV[i][:], func=AF.Square),
                             reads=[b_QKV[i]], writes=[bT['sqo']])
                        pb, b_pb = gbank()
                        S.op('pe', lambda e, pb=pb: e.matmul(pb[:], lhsT=ones_f[:], rhs=T['sqo'][:], start=True, stop=True),
                             reads=[b_c, bT['sqo']], writes=[b_pb])
                        S.op('dve', lambda e, pb=pb: e.tensor_scalar(out=T['rs'][:], in0=pb[:], scalar1=EPS, scalar2=None,
                                                                     op0=ALU.add), reads=[b_pb], writes=[bT['rs']])
                        S.op('act', lambda e: e.activation(out=T['rs'][:], in_=T['rs'][:], func=AF.Sqrt),
                             reads=[bT['rs']], writes=[bT['rs']])
                        S.op('dve', lambda e: e.reciprocal(out=T['rs'][:], in_=T['rs'][:]), reads=[bT['rs']], writes=[bT['rs']])
                        S.op('dve', lambda e, i=i: e.scalar_tensor_tensor(out=QKV[i][:], in0=QKV[i][:],
                                                                          scalar=(q_scale if i == 0 else 1.0), in1=T['rs'][:],
                                                                          op0=ALU.mult, op1=ALU.mult),
                             reads=[b_QKV[i], bT['rs']], writes=[b_QKV[i]])
                QT, KT, VT = QKV
                b_QT, b_KT, b_VT = b_QKV
                S.op('act', lambda e: e.activation(out=sc[:, 0, :], in_=ba[:, :, 0], func=AF.Sigmoid),
                     reads=[b_ba], writes=[b_sc])
                S.op('act', lambda e: e.activation(out=sc[:, 1, :], in_=ba[:, :, 1], func=AF.Exp, bias=dtb[:]),
                     reads=[b_ba, b_c], writes=[b_sc])
                S.op('act', lambda e: e.activation(out=sc[:, 1, :], in_=sc[:, 1, :], func=AF.Ln, bias=1.0),
                     reads=[b_sc], writes=[b_sc])
                S.op('dve', lambda e: e.tensor_scalar(out=sc[:, 1, :], in0=sc[:, 1, :], scalar1=negA[:], scalar2=None,
                                                      op0=ALU.mult), reads=[b_sc, b_c], writes=[b_sc])
                S.op('pe', lambda e: e.matmul(pS[:, 16:20], lhsT=Ui4[:, 0:128], rhs=sc[:, 1, :], start=True, stop=True),
                     reads=[b_c, b_sc], writes=[b_pS])
                S.op('dve', lambda e: e.tensor_copy(out=sc[:, 2, :], in_=pS[:, 16:20]), reads=[b_pS], writes=[b_sc])
                S.op('act', lambda e: e.activation(out=sc[:, 3, :], in_=sc[:, 2, :], func=AF.Exp), reads=[b_sc], writes=[b_sc])
                S.op('dve', lambda e: e.tensor_scalar(out=sc[:, 4, :], in0=sc[:, 0, :], scalar1=-1.0, scalar2=None, op0=ALU.mult),
                     reads=[b_sc], writes=[b_sc])
                S.op('dve', lambda e: e.tensor_tensor(out=sc[:, 5, :], in0=sc[:, 0, :], in1=sc[:, 3, :], op=ALU.mult),
                     reads=[b_sc], writes=[b_sc])
                dg4, tA4, tB4 = v4(T['diagG']), v4(T['tA']), v4(T['tB'])
                for n in range(4):
                    S.op('dve', lambda e, n=n: e.tensor_scalar(out=dg4[:, n, :], in0=ident_f, scalar1=sc[:, 2, n:n + 1],
                                                               scalar2=None, op0=ALU.mult),
                         reads=[b_c, b_sc], writes=[bT['diagG']])
                pR, b_pR = gbank()
                S.op('pe', lambda e: e.matmul(pR[:], lhsT=ones_f[:], rhs=T['diagG'][:], start=True, stop=True),
                     reads=[b_c, bT['diagG']], writes=[b_pR])
                pR4 = pR[:].rearrange('p (a b) -> p a b', a=4)
                for n in range(4):
                    S.op('dve', lambda e, n=n: e.tensor_scalar(out=tA4[:, n, :], in0=pR4[:, n, :], scalar1=sc[:, 2, n:n + 1],
                                                               scalar2=0.0, op0=ALU.subtract, op1=ALU.max),
                         reads=[b_pR, b_sc], writes=[bT['tA']])
                    S.op('dve', lambda e, n=n: e.tensor_scalar(out=tB4[:, n, :], in0=pR4[:, n, :], scalar1=sc[:, 2, n:n + 1],
                                                               scalar2=0.0, op0=ALU.subtract, op1=ALU.min),
                         reads=[b_pR, b_sc], writes=[bT['tB']])
                S.op('act', lambda e: e.activation(out=T['expR'][:], in_=pR[:], func=AF.Exp), reads=[b_pR], writes=[bT['expR']])
                S.op('act', lambda e: e.activation(out=T['tA'][:], in_=T['tA'][:], func=AF.Exp, scale=-1.0),
                     reads=[bT['tA']], writes=[bT['tA']])
                S.op('act', lambda e: e.activation(out=T['tB'][:], in_=T['tB'][:], func=AF.Exp),
                     reads=[bT['tB']], writes=[bT['tB']])
                S.op(POOL_ENG, lambda e: e.tensor_tensor(out=T['tA'][:], in0=T['tA'][:], in1=Ls4[:], op=ALU.mult),
                     reads=[bT['tA'], b_c], writes=[bT['tA']])
                S.op(POOL_ENG, lambda e: e.tensor_tensor(out=T['tB'][:], in0=T['tB'][:], in1=Ui4[:], op=ALU.mult),
                     reads=[bT['tB'], b_c], writes=[bT['tB']])
                dls, dT_ = T['tA'], T['tB']
                b_dls, b_dT = bT['tA'], bT['tB']
                dT4 = v4(dT_)
                eR4 = v4(T['expR'])
                pK, b_pK = gbank()
                for n in range(4):
                    S.op('pe', lambda e, n=n: e.transpose(out=pK[:, n * 128:(n + 1) * 128], in_=KT[:, n * 128:(n + 1) * 128],
                                                           identity=ident_f), reads=[b_KT, b_c], writes=[b_pK])
                pK4 = pK[:].rearrange('p (a b) -> p a b', a=4)
                Kbe4, Kdec4, Vb4 = v4(T['Kbe']), v4(T['Kdec']), v4(T['Vb'])
                for n in range(4):
                    S.op('dve', lambda e, n=n: e.tensor_scalar(out=Kbe4[:, n, :], in0=pK4[:, n, :], scalar1=sc[:, 5, n:n + 1],
                                                               scalar2=None, op0=ALU.mult),
                         reads=[b_pK, b_sc], writes=[bT['Kbe']])
                    S.op('act', lambda e, n=n: e.activation(out=Kdec4[:, n, :], in_=pK4[:, n, :], func=AF.Copy,
                                                            scale=dT4[:, n, 127:128]),
                         reads=[b_pK, b_dT], writes=[bT['Kdec']])
                pV, b_pV = gbank()
                for n in range(4):
                    S.op('pe', lambda e, n=n: e.transpose(out=pV[:, n * 128:(n + 1) * 128], in_=VT[:, n * 128:(n + 1) * 128],
                                                           identity=ident_f), reads=[b_VT, b_c], writes=[b_pV])
                pV4 = pV[:].rearrange('p (a b) -> p a b', a=4)
                for n in range(4):
                    S.op('dve', lambda e, n=n: e.tensor_scalar(out=Vb4[:, n, :], in0=pV4[:, n, :], scalar1=sc[:, 0, n:n + 1],
                                                               scalar2=None, op0=ALU.mult),
                         reads=[b_pV, b_sc], writes=[bT['Vb']])
                pKK, b_pKK = gbank()
                for n in range(4):
                    S.op('pe', lambda e, n=n: e.matmul(pKK[:, n * 128:(n + 1) * 128], lhsT=KT[:, n * 128:(n + 1) * 128],
                                                        rhs=KT[:, n * 128:(n + 1) * 128], start=True, stop=True),
                         reads=[b_KT], writes=[b_pKK])
                pKK4 = pKK[:].rearrange('p (a b) -> p a b', a=4)
                B = [T['B0'], T['B1']]
                bB = [bT['B0'], bT['B1']]
                BT = [T['BT0'], T['BT1']]
                bBT = [bT['BT0'], bT['BT1']]
                B04 = v4(B[0])
                dls4 = v4(dls)
                for n in range(4):
                    S.op('dve', lambda e, n=n: e.scalar_tensor_tensor(out=B04[:, n, :], in0=pKK4[:, n, :],
                                                                      scalar=sc[:, 4, n:n + 1], in1=dls4[:, n, :],
                                                                      op0=ALU.mult, op1=ALU.mult),
                         reads=[b_pKK, b_sc, b_dls], writes=[bB[0]])
                pKQ, b_pKQ = gbank()
                for n in range(4):
                    S.op('pe', lambda e, n=n: e.matmul(pKQ[:, n * 128:(n + 1) * 128], lhsT=KT[:, n * 128:(n + 1) * 128],
                                                        rhs=QT[:, n * 128:(n + 1) * 128], start=True, stop=True),
                         reads=[b_KT, b_QT], writes=[b_pKQ])
                S.op('dve', lambda e: e.tensor_tensor(out=T['qkT'][:], in0=pKQ[:], in1=dT_[:], op=ALU.mult),
                     reads=[b_pKQ, b_dT], writes=[bT['qkT']])
                S.op(POOL_ENG, lambda e: e.tensor_tensor(out=T['qdT'][:], in0=QT[:], in1=T['expR'][:], op=ALU.mult),
                     reads=[b_QT, bT['expR']], writes=[bT['qdT']])
                pb, b_pb = gbank()
                for n in range(4):
                    S.op('pe', lambda e, n=n, pb=pb: e.transpose(out=pb[:, n * 128:(n + 1) * 128],
                                                                  in_=B[0][:, n * 128:(n + 1) * 128], identity=ident_f),
                         reads=[bB[0], b_c], writes=[b_pb])
                S.op('act', lambda e, pb=pb: e.activation(out=BT[0][:], in_=pb[:], func=AF.Copy), reads=[b_pb], writes=[bBT[0]])
                S.op('dve', lambda e, pb=pb: e.tensor_tensor(out=T['TT'][:], in0=pb[:], in1=ident4[:], op=ALU.add),
                     reads=[b_pb, b_c], writes=[bT['TT']])
                cur = 0
                for lvl in range(1, 7):
                    nxt = 1 - cur
                    pb, b_pb = gbank()
                    for n in range(4):
                        cs = slice(n * 128, (n + 1) * 128)
                        S.op('pe', lambda e, cs=cs, pb=pb, cur=cur: e.matmul(pb[:, cs], lhsT=BT[cur][:, cs], rhs=B[cur][:, cs],
                                                                              start=True, stop=True),
                             reads=[bBT[cur], bB[cur]], writes=[b_pb])
                    if lvl < 6:
                        pb2, b_pb2 = gbank()
                        for n in range(4):
                            cs = slice(n * 128, (n + 1) * 128)
                            S.op('pe', lambda e, cs=cs, pb2=pb2, cur=cur: e.matmul(pb2[:, cs], lhsT=B[cur][:, cs],
                                                                                    rhs=BT[cur][:, cs], start=True, stop=True),
                                 reads=[bBT[cur], bB[cur]], writes=[b_pb2])
                    S.op('act', lambda e, pb=pb, nxt=nxt: e.activation(out=B[nxt][:], in_=pb[:], func=AF.Copy),
                         reads=[b_pb], writes=[bB[nxt]])
                    if lvl < 6:
                        S.op('act', lambda e, pb2=pb2, nxt=nxt: e.activation(out=BT[nxt][:], in_=pb2[:], func=AF.Copy),
                             reads=[b_pb2], writes=[bBT[nxt]])
                    pb3, b_pb3 = gbank()
                    for n in range(4):
                        cs = slice(n * 128, (n + 1) * 128)
                        S.op('pe', lambda e, cs=cs, pb3=pb3, nxt=nxt: e.matmul(pb3[:, cs], lhsT=B[nxt][:, cs], rhs=T['TT'][:, cs],
                                                                                start=True, stop=True),
                             reads=[bB[nxt], bT['TT']], writes=[b_pb3])
                    S.op('dve', lambda e, pb3=pb3: e.tensor_tensor(out=T['TT'][:], in0=pb3[:], in1=T['TT'][:], op=ALU.add),
                         reads=[b_pb3, bT['TT']], writes=[bT['TT']])
                    cur = nxt
                pb, b_pb = gbank()
                for n in range(4):
                    cs = slice(n * 128, (n + 1) * 128)
                    S.op('pe', lambda e, cs=cs, pb=pb: e.matmul(pb[:, cs], lhsT=T['TT'][:, cs], rhs=T['Vb'][:, cs],
                                                                 start=True, stop=True),
                         reads=[bT['TT'], bT['Vb']], writes=[b_pb])
                S.op('act', lambda e, pb=pb: e.activation(out=T['u'][:], in_=pb[:], func=AF.Copy), reads=[b_pb], writes=[bT['u']])
                pb, b_pb = gbank()
                for n in range(4):
                    cs = slice(n * 128, (n + 1) * 128)
                    S.op('pe', lambda e, cs=cs, pb=pb: e.matmul(pb[:, cs], lhsT=T['Kbe'][:, cs], rhs=T['TT'][:, cs],
                                                                 start=True, stop=True),
                         reads=[bT['TT'], bT['Kbe']], writes=[b_pb])
                S.op('act', lambda e, pb=pb: e.activation(out=T['wT'][:], in_=pb[:], func=AF.Copy), reads=[b_pb], writes=[bT['wT']])
                for n in range(4):
                    cs = slice(n * 128, (n + 1) * 128)
                    S.op('pe', lambda e, cs=cs: e.matmul(pS[:, 128:256], lhsT=T['wT'][:, cs], rhs=Sst[:], start=True, stop=True),
                         reads=[bT['wT'], b_Sst], writes=[b_pS])
                    S.op('dve', lambda e, cs=cs: e.tensor_tensor(out=vnew[:], in0=T['u'][:, cs], in1=pS[:, 128:256],
                                                                 op=ALU.subtract), reads=[bT['u'], b_pS], writes=[b_vnew])
                    S.op('pe', lambda e, cs=cs: e.matmul(pO[:, cs], lhsT=Sst[:], rhs=T['qdT'][:, cs], start=True, stop=False),
                         reads=[b_Sst, bT['qdT']], writes=[b_pO])
                    S.op('pe', lambda e, cs=cs: e.matmul(pO[:, cs], lhsT=vnew[:], rhs=T['qkT'][:, cs], start=False, stop=True),
                         reads=[b_vnew, bT['qkT']], writes=[b_pO])
                    S.op('pe', lambda e, cs=cs: e.matmul(pS[:, 256:384], lhsT=T['Kdec'][:, cs], rhs=vnew[:], start=True, stop=True),
                         reads=[bT['Kdec'], b_vnew], writes=[b_pS])
                    S.op('dve', lambda e, n=n: e.scalar_tensor_tensor(out=Sst[:], in0=Sst[:], scalar=eR4[:, n, 127:128],
                                                                      in1=pS[:, 256:384], op0=ALU.mult, op1=ALU.add),
                         reads=[b_Sst, bT['expR'], b_pS], writes=[b_Sst])
                S.op('act', lambda e: e.activation(out=T['sqo'][:], in_=pO[:], func=AF.Square), reads=[b_pO], writes=[bT['sqo']])
                pb, b_pb = gbank()
                S.op('pe', lambda e, pb=pb: e.matmul(pb[:], lhsT=ones_f[:], rhs=T['sqo'][:], start=True, stop=True),
                     reads=[b_c, bT['sqo']], writes=[b_pb])
                S.op('dve', lambda e, pb=pb: e.tensor_scalar(out=T['rs'][:], in0=pb[:], scalar1=1.0 / 128, scalar2=EPS,
                                                             op0=ALU.mult, op1=ALU.add), reads=[b_pb], writes=[bT['rs']])
                S.op('act', lambda e: e.activation(out=T['rs'][:], in_=T['rs'][:], func=AF.Sqrt), reads=[bT['rs']], writes=[bT['rs']])
                S.op('dve', lambda e: e.reciprocal(out=T['rs'][:], in_=T['rs'][:]), reads=[bT['rs']], writes=[bT['rs']])
                S.op('dve', lambda e: e.scalar_tensor_tensor(out=T['dout'][:], in0=pO[:], scalar=gdo[:], in1=T['rs'][:],
                                                             op0=ALU.mult, op1=ALU.mult),
                     reads=[b_pO, b_c, bT['rs']], writes=[bT['dout']])
                if out_dtype == F32:
                    S.op(POOL_ENG, lambda e: e.tensor_tensor(out=T['dout'][:], in0=T['dout'][:], in1=zs[:], op=ALU.mult),
                         reads=[bT['dout'], b_zs], writes=[bT['dout']])
                    S.dma('sp', lambda e: e.dma_start(out=deltaT[:, tok0:tok0 + SUB], in_=T['dout'][:]), reads=[bT['dout']])
                else:
                    dob = T['sqo'][:].bitcast(BF16)[:, 0:SUB]
                    S.op(POOL_ENG, lambda e: e.tensor_tensor(out=dob, in0=T['dout'][:], in1=zs[:], op=ALU.mult),
                         reads=[bT['dout'], b_zs], writes=[bT['sqo']])
                    S.dma('sp', lambda e: e.dma_start(out=deltaT[:, tok0:tok0 + SUB], in_=dob), reads=[bT['sqo']])
            if not do_attn:
                continue
            for p, d in enumerate(PATTERNS):
                nb = 16 // d
                for g in range(4):
                    pb, b_pb = gbank()
                    for q in range(4):
                        ti = g * 4 + q
                        r, n = ti // nb, ti % nb
                        S.op('pe', lambda e, q=q, pb=pb, r=r, n=n, d=d: e.transpose(
                            out=pb[:, q * 128:(q + 1) * 128], in_=vaT[:, sl(n * 128 * d + r, 128, d)], identity=ident_f),
                             reads=[b_vaT, b_c], writes=[b_pb])
                    S.op('act', lambda e, pb=pb, g=g, p=p: e.activation(out=Vt[p][par][:, g * 4:(g + 1) * 4, :].rearrange('p a b -> p (a b)'),
                                                                         in_=pb[:], func=AF.Copy),
                         reads=[b_pb], writes=[b_Vt[p][par]])
            tcount = 0
            for p, d in enumerate(PATTERNS):
                nb = 16 // d
                for ti in range(16):
                    r, n = ti // nb, ti % nb
                    qs = sl(n * 128 * d + r, 128, d)
                    has_prev = not (sb == 0 and n == 0)
                    Wd_ = 256 if has_prev else 128
                    if n > 0:
                        kprev = kaT[par][:, sl((n - 1) * 128 * d + r, 128, d)]
                        vprev = Vt[p][par][:, ti - 1, :]
                        rd_prev = [b_kaT[par], b_Vt[p][par]]
                    else:
                        kprev = kaT[1 - par][:, sl((nb - 1) * 128 * d + r, 128, d)]
                        vprev = Vt[p][1 - par][:, r * nb + nb - 1, :]
                        rd_prev = [b_kaT[1 - par], b_Vt[p][1 - par]]
                    pst, b_pst = gbank()
                    S.op('pe', lambda e, pst=pst, qs=qs: e.matmul(pst[:, 0:128], lhsT=kaT[par][:, qs], rhs=qaT[:, qs],
                                                                   start=True, stop=True),
                         reads=[b_kaT[par], b_qaT], writes=[b_pst])
                    if has_prev:
                        S.op('pe', lambda e, pst=pst, qs=qs, kprev=kprev: e.matmul(pst[:, 128:256], lhsT=kprev, rhs=qaT[:, qs],
                                                                                    start=True, stop=True),
                             reads=[rd_prev[0], b_qaT], writes=[b_pst])
                    a = tcount % 2
                    tcount += 1
                    S.op('dve', lambda e, pst=pst, a=a, p=p, Wd_=Wd_: e.tensor_tensor(out=atmp[a][:, 0:Wd_], in0=pst[:, 0:Wd_],
                                                                                      in1=ab[:, p, 0:Wd_], op=ALU.add),
                         reads=[b_pst, b_c], writes=[b_atmp[a]])
                    S.op('act', lambda e, a=a, Wd_=Wd_: e.activation(out=aPT[a][:, 0:Wd_], in_=atmp[a][:, 0:Wd_], func=AF.Exp),
                         reads=[b_atmp[a]], writes=[b_aPT[a]])
                    pod, b_pod = gbank()
                    S.op('pe', lambda e, pod=pod, a=a, p=p, ti=ti: e.matmul(pod[:, 0:128], lhsT=Vt[p][par][:, ti, :],
                                                                             rhs=aPT[a][:, 0:128], start=True, stop=not has_prev),
                         reads=[b_Vt[p][par], b_aPT[a]], writes=[b_pod])
                    if has_prev:
                        S.op('pe', lambda e, pod=pod, a=a, vprev=vprev: e.matmul(pod[:, 0:128], lhsT=vprev, rhs=aPT[a][:, 128:256],
                                                                                  start=False, stop=True),
                             reads=[rd_prev[1], b_aPT[a]], writes=[b_pod])
                    S.op('pe', lambda e, pod=pod, a=a: e.matmul(pod[:, 128:256], lhsT=ones_bf[:], rhs=aPT[a][:, 0:128],
                                                                 start=True, stop=not has_prev),
                         reads=[b_c, b_aPT[a]], writes=[b_pod])
                    if has_prev:
                        S.op('pe', lambda e, pod=pod, a=a: e.matmul(pod[:, 128:256], lhsT=ones_bf[:], rhs=aPT[a][:, 128:256],
                                                                     start=False, stop=True),
                             reads=[b_c, b_aPT[a]], writes=[b_pod])
                    pod2 = pod[:, 0:256].rearrange('p (a b) -> p a b', a=2)
                    if p == 0:
                        S.op('dve', lambda e, pod2=pod2, qs=qs: e.tensor_copy(out=acc[:, :, qs], in_=pod2),
                             reads=[b_pod], writes=[b_acc])
                    else:
                        S.op('dve', lambda e, pod2=pod2, qs=qs: e.tensor_tensor(out=acc[:, :, qs], in0=pod2, in1=acc[:, :, qs],
                                                                                op=ALU.add),
                             reads=[b_pod, b_acc], writes=[b_acc])
            S.op('dve', lambda e: e.reciprocal(out=acc[:, 1, :], in_=acc[:, 1, :]), reads=[b_acc], writes=[b_acc])
            if out_dtype == F32:
                S.op('dve', lambda e: e.tensor_tensor(out=acc[:, 0, :], in0=acc[:, 0, :], in1=acc[:, 1, :], op=ALU.mult),
                     reads=[b_acc], writes=[b_acc])
                S.dma('sp', lambda e, sb=sb: e.dma_start(out=attnT[:, sb * SB:(sb + 1) * SB], in_=acc[:, 0, :]), reads=[b_acc])
            else:
                aob = acc[:, 1, :].bitcast(BF16)[:, 0:SB]
                S.op('dve', lambda e: e.tensor_tensor(out=aob, in0=acc[:, 0, :], in1=acc[:, 1, :], op=ALU.mult),
                     reads=[b_acc], writes=[b_acc])
                S.dma('sp', lambda e, sb=sb: e.dma_start(out=attnT[:, sb * SB:(sb + 1) * SB], in_=aob), reads=[b_acc])
        S.build()
    return nc


def phase_a_inputs(inp):
    x = np.ascontiguousarray(inp['x'][0])
    w_in = inp['w_in'][0]
    cwf = inp['conv_w'][0]
    ii = np.arange(128)
    diff = ii[None, :] - ii[:, None]
    lstrict = (ii[:, None] > ii[None, :]).astype(np.float32)
    uincl = (ii[None, :] >= ii[:, None]).astype(np.float32)
    ident = np.eye(128, dtype=np.float32)
    maps = []
    for c in range(NCORE):
        hs = slice(c * 128, (c + 1) * 128)
        cols = [w_in[:, 0 * 1024 + c * 128:0 * 1024 + (c + 1) * 128],
                w_in[:, 1 * 1024 + c * 128:1 * 1024 + (c + 1) * 128],
                w_in[:, 2 * 1024 + c * 128:2 * 1024 + (c + 1) * 128],
                w_in[:, 3 * 1024 + c * 128:3 * 1024 + (c + 1) * 128],
                w_in[:, 4 * 1024 + c * 128:4 * 1024 + (c + 1) * 128],
                w_in[:, 5 * 1024 + c * 128:5 * 1024 + (c + 1) * 128],
                w_in[:, 6 * 1024 + c * 128:6 * 1024 + (c + 1) * 128],
                w_in[:, 7168 + c:7168 + c + 1],
                w_in[:, 7176 + c:7176 + c + 1]]
        w_sel = np.ascontiguousarray(np.concatenate(cols, axis=1))
        cw = np.zeros((128, 12), np.float32)
        for i in range(3):
            cw[:, i * 4:(i + 1) * 4] = cwf[:, i * 1024 + c * 128:i * 1024 + (c + 1) * 128].T
        slope = np.float32(2.0 ** (-8.0 * (c + 1) / 8.0))
        ab = np.zeros((128, 3, 256), np.float32)
        for p, d in enumerate(PATTERNS):
            cur = np.where(diff >= 0, -slope * d * diff.astype(np.float32), NEG)
            dprev = diff + 128
            prev = np.where(dprev <= 128, -slope * d * dprev.astype(np.float32), NEG)
            ab[:, p, 0:128] = cur
            ab[:, p, 128:256] = prev
        maps.append({
            'x': x, 'w_sel': w_sel, 'g_mix': _cols(inp['g_mix'][0], 16), 'conv_w': cw,
            'a_log': np.full((128, 1), inp['a_log'][0, c], np.float32),
            'dt_b': np.full((128, 1), inp['dt_bias'][0, c], np.float32),
            'g_do': np.ascontiguousarray(inp['g_delta_out'][0].reshape(128, 1)),
            'abias': ab, 'lstrict': np.tile(lstrict, (1, 4)), 'uincl': np.tile(uincl, (1, 4)),
            'ident4': np.tile(ident, (1, 4)),
        })
    return maps


def kernel(**inputs):
    inp = {k: np.asarray(v) for k, v in inputs.items()}
    cores = list(range(NCORE))
    nc_a = bass.Bass('TRN2', target_bir_lowering=False)
    build_phase_a(nc_a)
    res_a = run_bass_kernel_spmd(nc_a, phase_a_inputs(inp), core_ids=cores)
    mixT = np.stack([res_a.results[c]['attnT'] for c in cores] + [res_a.results[c]['deltaT'] for c in cores], axis=0)
    nc_b = bass.Bass('TRN2', target_bir_lowering=False)
    build_phase_b(nc_b)
    res_b = run_bass_kernel_spmd(nc_b, phase_b_inputs(inp, mixT), core_ids=cores)
    out = np.concatenate([res_b.results[c]['y'] for c in cores], axis=0)[None]
    return np.ascontiguousarray(out.astype(np.float32))
```
